# Optimizing a Trainium2 kernel written in Bass

```python
import math
import jax, jax.numpy as jnp
from jax import lax
import numpy as np


D_MODEL = 1024
BATCH = 8
SEQ = 4096
DEPTH = 1

SSD_EXPAND = 2
SSD_D_INNER = SSD_EXPAND * D_MODEL
SSD_HEAD_DIM = 64
SSD_N_HEADS = SSD_D_INNER // SSD_HEAD_DIM
SSD_N_GROUPS = 4
SSD_D_STATE = 128
SSD_CONV = 4
SSD_CHUNK = 128
SSD_CONV_DIM = SSD_D_INNER + 2 * SSD_N_GROUPS * SSD_D_STATE
ATT_HEAD_DIM = 64
ATT_N_HEADS = D_MODEL // ATT_HEAD_DIM
ATT_N_KV = 4
ATT_WINDOW = 128
ATT_BLOCK = 128
ATT_SCALE = ATT_HEAD_DIM ** -0.5
ROPE_THETA = 500000.0
ROPE_DIM = ATT_HEAD_DIM // 4
MOE_N_GROUPS = 8
MOE_EXPERTS_PER_GROUP = 8
MOE_N_EXPERTS = MOE_N_GROUPS * MOE_EXPERTS_PER_GROUP
MOE_TOP_K = 2
MOE_D_FF = 256
MOE_BLOCK = 128
RMS_EPS = 1e-6

COL_Z = SSD_D_INNER
COL_XBC = SSD_CONV_DIM
COL_DT = SSD_N_HEADS
COL_Q = ATT_N_HEADS * ATT_HEAD_DIM
COL_K = ATT_N_KV * ATT_HEAD_DIM
COL_V = ATT_N_KV * ATT_HEAD_DIM
COL_GATE = D_MODEL
IN_COLS = COL_Z + COL_XBC + COL_DT + COL_Q + COL_K + COL_V + 2 * COL_GATE
IN_SPLITS = (COL_Z,
             COL_Z + COL_XBC,
             COL_Z + COL_XBC + COL_DT,
             COL_Z + COL_XBC + COL_DT + COL_Q,
             COL_Z + COL_XBC + COL_DT + COL_Q + COL_K,
             COL_Z + COL_XBC + COL_DT + COL_Q + COL_K + COL_V,
             COL_Z + COL_XBC + COL_DT + COL_Q + COL_K + COL_V + COL_GATE)

kernel_name = 'hybrid_ssd_swa_sink_hmoe_block'


def rms_norm(t, g):
    tf = t.astype(jnp.float32)
    tf = tf * lax.rsqrt(jnp.mean(tf * tf, axis=-1, keepdims=True) + RMS_EPS)
    return (tf * g.astype(jnp.float32)).astype(t.dtype)


def ssd_branch(z, xbc, dt_raw, conv_w, conv_b, dt_bias, a_log, d_skip, ssd_norm_g):
    b, s, _ = xbc.shape
    G, J, P, N, L = SSD_N_GROUPS, SSD_N_HEADS // SSD_N_GROUPS, SSD_HEAD_DIM, SSD_D_STATE, SSD_CHUNK
    nc = s // L
    xbc = lax.conv_general_dilated(xbc, conv_w[:, None, :].astype(xbc.dtype), window_strides=(1,),
                                   padding=[(SSD_CONV - 1, 0)], dimension_numbers=('NWC', 'WIO', 'NWC'),
                                   feature_group_count=SSD_CONV_DIM) + conv_b
    xbc = jax.nn.silu(xbc)
    xs, bm, cm = jnp.split(xbc, [SSD_D_INNER, SSD_D_INNER + G * N], axis=-1)
    xs = xs.reshape(b, nc, L, G, J, P)
    bm = bm.reshape(b, nc, L, G, N)
    cm = cm.reshape(b, nc, L, G, N)
    dt = jax.nn.softplus(dt_raw.astype(jnp.float32) + dt_bias.astype(jnp.float32))
    a = -jnp.exp(a_log.astype(jnp.float32))
    dt_c = dt.reshape(b, nc, L, G, J)
    a_cum = jnp.cumsum(dt_c * a.reshape(G, J), axis=2)
    xdt = xs * dt_c[..., None].astype(xs.dtype)
    causal = (jnp.arange(L)[:, None] >= jnp.arange(L)[None, :])[None, None, :, :, None, None]
    seg = a_cum[:, :, :, None] - a_cum[:, :, None, :]
    decay = jnp.exp(jnp.where(causal, seg, -jnp.inf)).astype(xs.dtype)
    cb = jnp.einsum('bclgn,bcsgn->bclsg', cm, bm)
    y_diag = jnp.einsum('bclsgj,bcsgjp->bclgjp', cb[..., None] * decay, xdt)
    decay_to_end = jnp.exp(a_cum[:, :, -1:] - a_cum).astype(xs.dtype)
    states = jnp.einsum('bclgn,bclgjp->bcgjpn', bm, xdt * decay_to_end[..., None])
    chunk_decay = jnp.exp(a_cum[:, :, -1]).astype(states.dtype)

    def step(carry, inp):
        st, dec = inp
        return carry * dec[..., None, None] + st, carry

    init = jnp.zeros((b, G, J, P, N), states.dtype)
    _, prev = lax.scan(step, init, (jnp.moveaxis(states, 1, 0), jnp.moveaxis(chunk_decay, 1, 0)))
    prev = jnp.moveaxis(prev, 0, 1)
    y_off = jnp.einsum('bclgn,bcgjpn->bclgjp', cm, prev) * jnp.exp(a_cum)[..., None].astype(xs.dtype)
    y = (y_diag + y_off + xs * d_skip.reshape(G, J)[:, :, None].astype(xs.dtype)).reshape(b, s, SSD_D_INNER)
    yz = (y * jax.nn.silu(z)).reshape(b, s, G, SSD_D_INNER // G)
    return rms_norm(yz, ssd_norm_g.reshape(G, -1)).reshape(b, s, SSD_D_INNER)


def partial_rope(t, cos, sin):
    half = ROPE_DIM // 2
    t1, t2, rest = t[..., :half], t[..., half:ROPE_DIM], t[..., ROPE_DIM:]
    cos = cos.astype(t.dtype)
    sin = sin.astype(t.dtype)
    return jnp.concatenate([t1 * cos - t2 * sin, t2 * cos + t1 * sin, rest], axis=-1)


def swa_sink_branch(q, k, v, positions, q_norm_g, k_norm_g, sinks):
    b, s, _ = q.shape
    HD, KV, QG, BLK = ATT_HEAD_DIM, ATT_N_KV, ATT_N_HEADS // ATT_N_KV, ATT_BLOCK
    nb = s // BLK
    q = rms_norm(q.reshape(b, s, ATT_N_HEADS, HD), q_norm_g)
    k = rms_norm(k.reshape(b, s, KV, HD), k_norm_g)
    v = v.reshape(b, s, KV, HD)
    inv_freq = ROPE_THETA ** (-jnp.arange(0, ROPE_DIM, 2, dtype=jnp.float32) / ROPE_DIM)
    ang = positions.astype(jnp.float32)[..., None] * inv_freq
    cos, sin = jnp.cos(ang)[:, :, None, :], jnp.sin(ang)[:, :, None, :]
    q = partial_rope(q, cos, sin)
    k = partial_rope(k, cos, sin)
    qb = q.reshape(b, nb, BLK, KV, QG, HD)
    kb = k.reshape(b, nb, BLK, KV, HD)
    vb = v.reshape(b, nb, BLK, KV, HD)
    pad = ((0, 0), (1, 0), (0, 0), (0, 0), (0, 0))
    kk = jnp.concatenate([jnp.pad(kb, pad)[:, :-1], kb], axis=2)
    vv = jnp.concatenate([jnp.pad(vb, pad)[:, :-1], vb], axis=2)
    scores = jnp.einsum('bnqhgd,bnkhd->bnhgqk', qb, kk).astype(jnp.float32) * ATT_SCALE
    qi = jnp.arange(nb)[:, None, None] * BLK + jnp.arange(BLK)[None, :, None]
    ki = (jnp.arange(nb)[:, None, None] - 1) * BLK + jnp.arange(2 * BLK)[None, None, :]
    diff = qi - ki
    mask = (diff >= 0) & (diff < ATT_WINDOW) & (ki >= 0)
    scores = jnp.where(mask[None, :, None, None], scores, -jnp.inf)
    sink = sinks.astype(jnp.float32).reshape(KV, QG)[None, None, :, :, None]
    m = jnp.maximum(jnp.max(scores, axis=-1), sink)
    p = jnp.exp(scores - m[..., None])
    denom = jnp.sum(p, axis=-1) + jnp.exp(sink - m)
    p = (p / denom[..., None]).astype(v.dtype)
    out = jnp.einsum('bnhgqk,bnkhd->bnqhgd', p, vv)
    return out.reshape(b, s, ATT_N_HEADS * HD)


def hier_moe(h2, w_rg, b_rg, w_re, b_re, w_gate_e, w_up_e, w_down_e):
    t = h2.shape[0]
    rows = jnp.arange(t)
    g_logits = jnp.dot(h2, w_rg).astype(jnp.float32) + b_rg.astype(jnp.float32)
    g_prob = jax.nn.softmax(g_logits, axis=-1)
    g_sel = jnp.argmax(g_logits, axis=-1).astype(jnp.int32)
    p_group = g_prob[rows, g_sel]
    e_logits_all = jnp.einsum('td,gde->tge', h2, w_re).astype(jnp.float32) + b_re.astype(jnp.float32)
    e_logits = e_logits_all[rows, g_sel]
    top_v, top_i = lax.top_k(e_logits, MOE_TOP_K)
    gate = p_group[:, None] * jax.nn.softmax(top_v, axis=-1)
    expert_id = g_sel[:, None] * MOE_EXPERTS_PER_GROUP + top_i.astype(jnp.int32)
    n_assign = t * MOE_TOP_K
    flat_e = expert_id.reshape(-1)
    flat_w = gate.reshape(-1)
    flat_t = jnp.arange(n_assign, dtype=jnp.int32) // MOE_TOP_K
    order = jnp.argsort(flat_e)
    sorted_e = flat_e[order]
    counts = jnp.bincount(flat_e, length=MOE_N_EXPERTS)
    starts = jnp.cumsum(counts) - counts
    padded = (counts + MOE_BLOCK - 1) // MOE_BLOCK * MOE_BLOCK
    padded_end = jnp.cumsum(padded)
    padded_start = padded_end - padded
    dest = padded_start[sorted_e] + jnp.arange(n_assign) - starts[sorted_e]
    n_rows = n_assign + MOE_N_EXPERTS * MOE_BLOCK
    n_blocks = n_rows // MOE_BLOCK
    tok = jnp.zeros((n_rows,), jnp.int32).at[dest].set(flat_t[order])
    wt = jnp.zeros((n_rows,), jnp.float32).at[dest].set(flat_w[order])
    block_e = jnp.minimum(jnp.searchsorted(padded_end, jnp.arange(n_blocks) * MOE_BLOCK, side='right'),
                          MOE_N_EXPERTS - 1)
    xs = h2[tok].reshape(n_blocks, MOE_BLOCK, -1)

    def expert_block(args):
        xb, e = args
        hid = jax.nn.silu(xb @ w_gate_e[e]) * (xb @ w_up_e[e])
        return hid @ w_down_e[e]

    ys = lax.map(expert_block, (xs, block_e)).reshape(n_rows, -1)
    return jnp.zeros_like(h2).at[tok].add(ys * wt[:, None].astype(ys.dtype))


def hybrid_layer(x, positions, norm1_g, w_in, conv_w, conv_b, dt_bias, a_log, d_skip, ssd_norm_g,
                 w_ssd_out, q_norm_g, k_norm_g, sinks, w_attn_out, w_out, norm2_g, w_router_group,
                 b_router_group, w_router_expert, b_router_expert, w_gate_e, w_up_e, w_down_e):
    b, s, d = x.shape
    h = rms_norm(x, norm1_g)
    proj = h @ w_in
    z, xbc, dt_raw, q, k, v, g_ssd, g_att = jnp.split(proj, IN_SPLITS, axis=-1)
    y_ssd = ssd_branch(z, xbc, dt_raw, conv_w, conv_b, dt_bias, a_log, d_skip, ssd_norm_g) @ w_ssd_out
    y_att = swa_sink_branch(q, k, v, positions, q_norm_g, k_norm_g, sinks) @ w_attn_out
    merged = jax.nn.sigmoid(g_ssd) * y_ssd + jax.nn.sigmoid(g_att) * y_att
    x = x + merged @ w_out
    h2 = rms_norm(x, norm2_g).reshape(b * s, d)
    y_moe = hier_moe(h2, w_router_group, b_router_group, w_router_expert, b_router_expert,
                     w_gate_e, w_up_e, w_down_e)
    return x + y_moe.reshape(b, s, d)


def setup_inputs(seed: int = 0) -> dict:
    key = jax.random.key(seed)
    ks = jax.random.split(key, 24)
    L = DEPTH

    def nrm(k, shape, scale):
        return jax.random.normal(k, shape, jnp.float32) * scale

    x = nrm(ks[0], (BATCH, SEQ, D_MODEL), 1.0)
    positions = (jax.random.randint(ks[1], (BATCH, 1), 0, 1024, dtype=jnp.int32)
                 + jnp.arange(SEQ, dtype=jnp.int32)[None, :])
    dt0 = jnp.exp(jax.random.uniform(ks[6], (L, SSD_N_HEADS), jnp.float32,
                                     minval=math.log(1e-3), maxval=math.log(1e-1)))
    return {
        'x': x,
        'positions': positions,
        'norm1_g': 1.0 + nrm(ks[2], (L, D_MODEL), 0.02),
        'w_in': nrm(ks[3], (L, D_MODEL, IN_COLS), D_MODEL ** -0.5),
        'conv_w': nrm(ks[4], (L, SSD_CONV, SSD_CONV_DIM), SSD_CONV ** -0.5),
        'conv_b': nrm(ks[5], (L, SSD_CONV_DIM), 0.02),
        'dt_bias': dt0 + jnp.log(-jnp.expm1(-dt0)),
        'a_log': jnp.log(jax.random.uniform(ks[7], (L, SSD_N_HEADS), jnp.float32, minval=1.0, maxval=16.0)),
        'd_skip': 1.0 + nrm(ks[8], (L, SSD_N_HEADS), 0.1),
        'ssd_norm_g': 1.0 + nrm(ks[9], (L, SSD_D_INNER), 0.02),
        'w_ssd_out': nrm(ks[10], (L, SSD_D_INNER, D_MODEL), SSD_D_INNER ** -0.5),
        'q_norm_g': 1.0 + nrm(ks[11], (L, ATT_HEAD_DIM), 0.02),
        'k_norm_g': 1.0 + nrm(ks[12], (L, ATT_HEAD_DIM), 0.02),
        'sinks': nrm(ks[13], (L, ATT_N_HEADS), 0.5),
        'w_attn_out': nrm(ks[14], (L, ATT_N_HEADS * ATT_HEAD_DIM, D_MODEL), (ATT_N_HEADS * ATT_HEAD_DIM) ** -0.5),
        'w_out': nrm(ks[15], (L, D_MODEL, D_MODEL), D_MODEL ** -0.5),
        'norm2_g': 1.0 + nrm(ks[16], (L, D_MODEL), 0.02),
        'w_router_group': nrm(ks[17], (L, D_MODEL, MOE_N_GROUPS), D_MODEL ** -0.5),
        'b_router_group': nrm(ks[18], (L, MOE_N_GROUPS), 0.01),
        'w_router_expert': nrm(ks[19], (L, MOE_N_GROUPS, D_MODEL, MOE_EXPERTS_PER_GROUP), D_MODEL ** -0.5),
        'b_router_expert': nrm(ks[20], (L, MOE_N_GROUPS, MOE_EXPERTS_PER_GROUP), 0.01),
        'w_gate_e': nrm(ks[21], (L, MOE_N_EXPERTS, D_MODEL, MOE_D_FF), D_MODEL ** -0.5),
        'w_up_e': nrm(ks[22], (L, MOE_N_EXPERTS, D_MODEL, MOE_D_FF), D_MODEL ** -0.5),
        'w_down_e': nrm(ks[23], (L, MOE_N_EXPERTS, MOE_D_FF, D_MODEL), MOE_D_FF ** -0.5),
    }


def reference(x, positions, norm1_g, w_in, conv_w, conv_b, dt_bias, a_log, d_skip, ssd_norm_g,
              w_ssd_out, q_norm_g, k_norm_g, sinks, w_attn_out, w_out, norm2_g, w_router_group,
              b_router_group, w_router_expert, b_router_expert, w_gate_e, w_up_e, w_down_e):
    for l in range(DEPTH):
        x = hybrid_layer(x, positions, norm1_g[l], w_in[l], conv_w[l], conv_b[l], dt_bias[l], a_log[l],
                         d_skip[l], ssd_norm_g[l], w_ssd_out[l], q_norm_g[l], k_norm_g[l], sinks[l],
                         w_attn_out[l], w_out[l], norm2_g[l], w_router_group[l], b_router_group[l],
                         w_router_expert[l], b_router_expert[l], w_gate_e[l], w_up_e[l], w_down_e[l])
    return x
```

```python
import math
import numpy as np
import concourse.bass as bass
import concourse.mybir as mybir
from concourse.bass_utils import run_bass_kernel_spmd

F32 = mybir.dt.float32
BF16 = mybir.dt.bfloat16
I32 = mybir.dt.int32
AF = mybir.ActivationFunctionType
ALU = mybir.AluOpType
AX = mybir.AxisListType

D = 1024
SEQ = 4096
NCHUNK = 32
INC = 8736
C_Z, C_XS, C_B, C_C, C_DT, C_Q, C_K, C_V, C_GS, C_GA = 0, 2048, 4096, 4608, 5120, 5152, 6176, 6432, 6688, 7712
NE = 64
CAP = 256
NSLOT = NE * CAP
EPS = 1e-6
BIG = 30000.0
R_DTB, R_ALOG, R_DSK, R_QKG, R_SINK, R_G2, R_RB, R_W = 0, 32, 64, 96, 1376, 1392, 2416, 2488


class T:
    def __init__(self, name=''):
        self.name = name
        self.w = None
        self.r = []


class Node:
    __slots__ = ('e', 'fn', 'dma', 'sync', 'order', 'dur', 'lat', 'succ', 'indeg', 'ready', 'start', 'fin', 'cnt', 'key', 'tbl', 'tag')

    def __init__(self, e, fn, dma, dur, lat):
        self.e = e; self.fn = fn; self.dma = dma; self.sync = set(); self.order = set(); self.dur = dur; self.lat = lat
        self.succ = []; self.indeg = 0; self.ready = 0.0; self.start = 0.0; self.fin = 0.0; self.cnt = 0; self.key = None; self.tbl = None


class Sched:
    ENG = ('pe', 'act', 'dve', 'pool', 'sp')

    def __init__(self, nc):
        self.nc = nc
        self.nodes = []
        self.last_dma = {}
        self.dsem = {}

    def op(self, e, fn, reads=(), writes=(), dma=None, dur=200.0, lat=0.0, tbl=None):
        nid = len(self.nodes)
        nd = Node(e, fn, dma, dur, lat)
        nd.tbl = tbl
        N = self.nodes
        for t in reads:
            if t.w is not None:
                nd.sync.add(t.w)
        for t in writes:
            if t.w is not None:
                pw = N[t.w]
                if e == 'pe' and pw.e == 'pe' and dma is None and pw.dma is None:
                    nd.order.add(t.w)
                elif dma is not None and pw.dma == dma:
                    nd.order.add(t.w)
                else:
                    nd.sync.add(t.w)
            for r in t.r:
                pr = N[r]
                if pr.e == e and dma is None and pr.dma is None and e == 'pe':
                    nd.order.add(r)
                else:
                    nd.sync.add(r)
        if dma is not None:
            if dma in self.last_dma:
                nd.order.add(self.last_dma[dma])
            self.last_dma[dma] = nid
        nd.order -= nd.sync
        N.append(nd)
        for t in reads:
            t.r.append(nid)
        for t in writes:
            t.w = nid
            t.r = []
        return nid

    def final_wait(self, e, tiles):
        nd = Node(e, None, None, 0.0, 0.0)
        for t in tiles:
            if t.w is not None:
                nd.sync.add(t.w)
        self.nodes.append(nd)

    def schedule(self):
        import heapq
        N = self.nodes
        for i, nd in enumerate(N):
            for d in nd.sync | nd.order:
                N[d].succ.append(i)
            nd.indeg = len(nd.sync | nd.order)
        waitq = {e: [] for e in self.ENG}
        readyq = {e: [] for e in self.ENG}
        free = {e: 0.0 for e in self.ENG}
        for i, nd in enumerate(N):
            if nd.indeg == 0:
                heapq.heappush(waitq[nd.e], (0.0, i))
        order = []
        remaining = len(N)
        cur_tbl = [None]
        self.n_tbl = 0
        dma_free = [0.0]
        while remaining:
            best = None
            for e in self.ENG:
                wq, rq = waitq[e], readyq[e]
                while wq and wq[0][0] <= free[e]:
                    heapq.heappush(rq, heapq.heappop(wq)[1])
                if rq:
                    cand = (free[e], rq[0], e, True)
                elif wq:
                    cand = (wq[0][0], wq[0][1], e, False)
                else:
                    continue
                if best is None or cand[:2] < best[:2]:
                    best = cand
            st, i, e, from_ready = best
            extra = 0.0
            if from_ready:
                if e == 'act':
                    rq = readyq[e]
                    popped = []
                    pick = None
                    while rq and len(popped) < 16:
                        j = heapq.heappop(rq)
                        popped.append(j)
                        if N[j].tbl is None or N[j].tbl == cur_tbl[0]:
                            pick = j
                            break
                    if pick is None or (st - N[popped[0]].ready) > 6000.0:
                        pick = popped[0]
                    for j in popped:
                        if j != pick:
                            heapq.heappush(rq, j)
                    i = pick
                else:
                    heapq.heappop(readyq[e])
            else:
                heapq.heappop(waitq[e])
            nd = N[i]
            if e == 'act' and nd.tbl is not None and nd.tbl != cur_tbl[0]:
                cur_tbl[0] = nd.tbl
                extra = 1300.0
                self.n_tbl += 1
            nd.start = st
            free[e] = st + nd.dur + extra
            nd.fin = st + nd.dur + extra + nd.lat
            order.append(i)
            remaining -= 1
            if nd.dma is not None:
                xfer = nd.lat - 2000.0
                x0 = max(st + nd.dur, dma_free[0])
                dma_free[0] = x0 + xfer * 0.5
                nd.fin = x0 + xfer * 0.5 + 2000.0
            for s_ in nd.succ:
                sn = N[s_]
                if i in sn.sync:
                    r_ = nd.fin + (self.hop if sn.e != e else 150.0)
                else:
                    r_ = nd.start
                if r_ > sn.ready:
                    sn.ready = r_
                sn.indeg -= 1
                if sn.indeg == 0:
                    heapq.heappush(waitq[sn.e], (sn.ready, s_))
        self.order = order
        self.est = max(free.values())
        return order

    def run(self):
        order = self.schedule()
        N = self.nodes
        nc = self.nc
        sem = {e: nc.alloc_semaphore('s_' + e) for e in self.ENG}
        cnt = {e: 0 for e in self.ENG}
        dcnt = {}
        clock = {e: {} for e in self.ENG}
        snap = {}
        prog = {e: [] for e in self.ENG}
        for i in order:
            nd = N[i]
            e = nd.e
            clk = clock[e]
            best = {}
            for d in nd.sync:
                k, v = N[d].key
                if clk.get(k, 0) >= v:
                    continue
                best[k] = max(best.get(k, 0), v)
                for kk, vv in snap[d].items():
                    if clk.get(kk, 0) < vv:
                        clk[kk] = vv
                if clk.get(k, 0) < v:
                    clk[k] = v
            if nd.fn is None:
                prog[e].append((list(best.items()), None, None))
                continue
            if nd.dma is None:
                cnt[e] += 1
                nd.key = (e, cnt[e])
            else:
                if nd.dma not in self.dsem:
                    self.dsem[nd.dma] = nc.alloc_semaphore('d_' + nd.dma)
                    dcnt[nd.dma] = 0
                dcnt[nd.dma] += 16
                nd.key = ('d:' + nd.dma, dcnt[nd.dma])
            snap[i] = dict(clk)
            prog[e].append((list(best.items()), nd.fn, nd.key))
        self.n_waits = sum(len(w) for e in self.ENG for (w, _, _) in prog[e])
        S = self

        def semof(k):
            return S.dsem[k[2:]] if k.startswith('d:') else sem[k]
        with nc.Block() as block:
            def mk(e):
                def f(engine):
                    for waits, fn, key in prog[e]:
                        for k, v in waits:
                            engine.wait_ge(semof(k), v)
                        if fn is None:
                            continue
                        ins = fn(engine)
                        ins.then_inc(semof(key[0]), 16 if key[0].startswith('d:') else 1)
                return f
            block.tensor(mk('pe'))
            block.scalar(mk('act'))
            block.vector(mk('dve'))
            block.gpsimd(mk('pool'))
            block.sync(mk('sp'))


def build(nsc=32, nexp=NE, NCH=1, dbg=False, model_only=False, opt=None):
    opt = opt or {}
    nc = bass.Bass("TRN2", target_bir_lowering=False)
    S = Sched(nc)
    TS = NCH * 128
    SR = opt.get('sring', 2)
    S.hop = opt.get('hop', 400.0)
    S.prio = opt.get('prio', 'nid')

    stage = ['setup']

    def fsz(ap):
        n = 1
        for d_ in list(ap.shape)[1:]:
            n *= int(d_)
        return n

    def OP(e, meth, reads, writes, *args, dma=None, **kw):
        lat = 0.0
        if dma is not None:
            o_ = kw.get('out')
            i_ = kw.get('in_')
            nbytes = min(fsz(o_) * int(o_.shape[0]), fsz(i_) * int(i_.shape[0])) * (2 if o_.dtype == BF16 else 4)
            dur = 1200.0 if e == 'pool' else 60.0
            lat = 2000.0 + nbytes / 150.0
        elif meth == 'matmul':
            o_ = args[0]
            n_ = fsz(o_)
            dur = 64.0 + (1.7 if kw['rhs'].dtype == F32 else 0.42) * n_
        elif meth == 'transpose':
            dur = 64.0 + (1.7 if args[1].dtype == F32 else 0.42) * 128
        else:
            o_ = kw.get('out', args[0] if args else None)
            n_ = fsz(o_)
            if e == 'act':
                dur = 220.0 + 0.75 * n_ + (100.0 if 'accum_out' in kw else 0.0)
            elif e == 'dve':
                dur = 120.0 + 1.05 * n_
            else:
                dur = 250.0 + 2.1 * n_
        import sys as _sys
        tag_ = '%s|%s:%d' % (stage[0], meth, _sys._getframe(1).f_lineno)
        tbl = None
        if meth == 'activation':
            tbl = {AF.Silu: 'silu', AF.Exp: 'exp', AF.Ln: 'ln', AF.Sin: 'silu'}.get(kw.get('func'))
        dur = dur * opt.get('scale', {}).get(e, 1.0)
        nid_ = S.op(e, lambda g: getattr(g, meth)(*args, **kw), reads, writes, dma, dur, lat, tbl)
        S.nodes[nid_].tag = tag_
        return nid_

    def dram_in(name, shape, dt=F32):
        return nc.dram_tensor(name, list(shape), dt, kind="ExternalInput").ap()

    x_d = dram_in("x", [SEQ, D])
    pos_d = dram_in("pos", [128, NCHUNK], I32)
    w_in_d = dram_in("w_in", [D, INC])
    w_so_d = dram_in("w_so", [2048, D])
    w_ao_d = dram_in("w_ao", [D, D])
    w_o_d = dram_in("w_o", [D, D])
    g1T_d = dram_in("g1T", [128, 8])
    convw_d = dram_in("convw", [128, 24, 4])
    convb_d = dram_in("convb", [128, 24])
    gssdT_d = dram_in("gssdT", [128, 16])
    rowc_d = dram_in("rowc", [128, R_W])
    wr_d = dram_in("wr", [128, 8, 72])
    wg_d = dram_in("wg", [NE, D, 256])
    wu_d = dram_in("wu", [NE, D, 256])
    wd_d = dram_in("wd", [NE, 256, D])
    out_d = nc.dram_tensor("out", [SEQ, D], F32, kind="ExternalOutput").ap()
    w_in_b = nc.dram_tensor("w_in_b", [D, INC], BF16, kind="Internal").ap()
    w_so_b = nc.dram_tensor("w_so_b", [2048, D], BF16, kind="Internal").ap()
    w_ao_b = nc.dram_tensor("w_ao_b", [D, D], BF16, kind="Internal").ap()
    w_o_b = nc.dram_tensor("w_o_b", [D, D], BF16, kind="Internal").ap()
    xs_scr = nc.dram_tensor("xs_scr", [NSLOT, D], BF16, kind="Internal").ap()
    ys_scr = nc.dram_tensor("ys_scr", [NSLOT, D], BF16, kind="Internal").ap()
    dbg_outs = {}

    class B:
        def __init__(self, name, shape, dt=F32):
            if model_only:
                self.ap = nc.dram_tensor("sb_" + name, list(shape), dt, kind="Internal").ap()
            else:
                self.ap = nc.alloc_sbuf_tensor("sb_" + name, list(shape), dt).ap()
            self.t = T(name)

    def ring(name, n, shape, dt=F32):
        return [B("%s%d" % (name, i), shape, dt) for i in range(n)]

    class V:
        def __init__(self, src_ap, src_t, c0, c1, shape, dt):
            ap2 = src_ap[:, c0:c1]
            if dt != src_ap.dtype:
                ap2 = ap2.bitcast(dt)
            self.ap = ap2 if len(shape) == 2 else ap2.rearrange("p (a b) -> p a b", a=shape[1])
            assert list(self.ap.shape) == list(shape), (self.ap.shape, shape)
            self.t = T()
            self.t.w = src_t.w
            self.t.r = list(src_t.r)

    psb = [nc.alloc_psum_tensor("psb%d" % i, [128, 512], F32).ap() for i in range(8)]
    pst = [T("ps%d" % i) for i in range(8)]

    ones_f = B("ones_f", [128, 128]); ident_f = B("ident_f", [128, 128]); ident_b = B("ident_b", [128, 128], BF16)
    ones_b = B("ones_b", [128, 128], BF16); ustr_b = B("ustr_b", [128, 128], BF16)
    maskle = B("maskle", [128, 128]); maskgt = B("maskgt", [128, 128])
    negb = B("negb", [128, 512], BF16); negcur = B("negcur", [128, 512], BF16); negprev = B("negprev", [128, 512], BF16)
    OP('pool', 'memset', [], [ones_f.t], ones_f.ap, 1.0)
    OP('pool', 'memset', [], [ones_b.t], ones_b.ap, 1.0)
    OP('pool', 'memset', [], [negb.t], negb.ap, -BIG)

    def asel(dst, src, pattern, cm, cmp):
        OP('pool', 'affine_select', [src.t], [dst.t], out=dst.ap, in_=src.ap, pattern=pattern, compare_op=cmp,
           fill=0.0, base=0, channel_multiplier=cm)
    asel(ident_f, ones_f, [[-1, 128]], 1, ALU.is_equal)
    asel(ident_b, ones_b, [[-1, 128]], 1, ALU.is_equal)
    asel(maskle, ones_f, [[1, 128]], -1, ALU.is_ge)
    asel(maskgt, ones_f, [[-1, 128]], 1, ALU.is_gt)
    asel(ustr_b, ones_b, [[1, 128]], -1, ALU.is_gt)
    asel(negcur, negb, [[0, 4], [-1, 128]], 1, ALU.is_gt)
    asel(negprev, negb, [[0, 4], [1, 128]], -1, ALU.is_ge)

    ebase_i = B("ebase_i", [128, 64], I32); ebase_b = B("ebase_b", [128, 64])
    OP('pool', 'iota', [], [ebase_i.t], ebase_i.ap, pattern=[[CAP, 64]], base=0, channel_multiplier=0)
    OP('dve', 'tensor_copy', [ebase_i.t], [ebase_b.t], out=ebase_b.ap, in_=ebase_i.ap)
    g1T = B("g1T", [128, 8]); convw = B("convw", [128, 24, 4]); convb = B("convb", [128, 24])
    gssdT = B("gssdT", [128, 16]); rowc = B("rowc", [128, R_W]); wr = B("wr", [128, 8, 72])
    pos_i = B("pos_i", [128, NCHUNK], I32)
    for b_, d_ in ((g1T, g1T_d), (convw, convw_d), (convb, convb_d), (gssdT, gssdT_d), (rowc, rowc_d), (wr, wr_d), (pos_i, pos_d)):
        OP('sp', 'dma_start', [], [b_.t], out=b_.ap, in_=d_, dma='c_' + b_.t.name)
    rc = rowc.ap
    a_neg = B("a_neg", [128, 32]); esink = B("esink", [128, 16])
    OP('act', 'activation', [rowc.t], [a_neg.t], out=a_neg.ap, in_=rc[:, R_ALOG:R_ALOG + 32], func=AF.Exp)
    OP('dve', 'tensor_scalar', [a_neg.t], [a_neg.t], a_neg.ap, a_neg.ap, -1.0, None, ALU.mult)
    OP('act', 'activation', [rowc.t], [esink.t], out=esink.ap, in_=rc[:, R_SINK:R_SINK + 16], func=AF.Exp)
    mrg = B("mrg", [128, D])
    posf = B("posf", [128, NCHUNK]); invf = B("invf", [128, 8]); ang = V(mrg.ap, mrg.t, 0, 256, [128, NCHUNK, 8], F32)
    cs_all = B("cs_all", [128, NCHUNK, 16])
    OP('dve', 'tensor_copy', [pos_i.t], [posf.t], out=posf.ap, in_=pos_i.ap)
    for j in range(8):
        OP('pool', 'memset', [], [invf.t], invf.ap[:, j:j + 1], float(500000.0 ** (-(2.0 * j) / 16.0)))
    OP('dve', 'tensor_tensor', [posf.t, invf.t], [ang.t], out=ang.ap,
       in0=posf.ap.unsqueeze(2).to_broadcast([128, NCHUNK, 8]), in1=invf.ap.unsqueeze(1).to_broadcast([128, NCHUNK, 8]), op=ALU.mult)
    angt = V(mrg.ap, mrg.t, 256, 512, [128, NCHUNK, 8], F32)
    angr = V(mrg.ap, mrg.t, 512, 768, [128, NCHUNK, 8], F32)
    MAGIC = 12582912.0
    for off, lo in ((0.25, 0), (0.0, 8)):
        OP('dve', 'tensor_scalar', [ang.t], [angt.t], angt.ap, ang.ap, 1.0 / (2.0 * math.pi), off, ALU.mult, ALU.add)
        OP('dve', 'tensor_scalar', [angt.t], [angr.t], angr.ap, angt.ap, MAGIC, None, ALU.add)
        OP('dve', 'tensor_scalar', [angr.t], [angr.t], angr.ap, angr.ap, -MAGIC, None, ALU.add)
        OP('dve', 'tensor_tensor', [angt.t, angr.t], [angt.t], out=angt.ap, in0=angt.ap, in1=angr.ap, op=ALU.subtract)
        OP('act', 'activation', [angt.t], [cs_all.t], out=cs_all.ap[:, :, lo:lo + 8], in_=angt.ap, func=AF.Sin, scale=2.0 * math.pi)

    SEGS = [(C_XS + i * 512, 512) for i in range(6)] + [(C_DT, 32)] + [(C_Z + i * 512, 512) for i in range(4)] + \
           [(C_GS + i * 512, 512) for i in range(4)] + [(C_Q, 512), (C_Q + 512, 512), (C_K, 512)]
    tw_seg = {}
    for si_, (c0_, n_) in enumerate(SEGS):
        tw_seg[c0_] = T("w_in_b%d" % c0_)
        OP('pool', 'dma_start', [], [tw_seg[c0_]], out=w_in_b[:, c0_:c0_ + n_], in_=w_in_d[:, c0_:c0_ + n_], dma='prep%d' % si_)
    tw_so = T("w_so_b"); tw_ao = T("w_ao_b"); tw_o = T("w_o_b")
    for (src, dst, tt, rows, nm_) in ((w_so_d, w_so_b, tw_so, 2048, 'pso'), (w_ao_d, w_ao_b, tw_ao, D, 'pao'), (w_o_d, w_o_b, tw_o, D, 'po')):
        for r0 in range(0, rows, 256):
            OP('pool', 'dma_start', [], [tt], out=dst[r0:r0 + 256, :], in_=src[r0:r0 + 256, :], dma=nm_)

    wbuf = ring("wbuf", opt.get("wring", 3), [128, 4096], BF16)
    wctr = [0]

    def wload(view_fn, src_ap, src_t):
        b_ = wbuf[wctr[0] % len(wbuf)]
        wctr[0] += 1
        eng = 'sp'
        OP(eng, 'dma_start', [src_t], [b_.t], out=view_fn(b_.ap), in_=src_ap, dma=b_.t.name)
        return b_

    xt_r = ring("xt", SR, [128, NCH, D]); xn = B("xn", [128, D], BF16)
    st1 = B("st1", [128, 8]); st2 = B("st2", [128, 8]); n1a = B("n1a", [128, 8]); n1b = B("n1b", [128, 8]); n2a = B("n2a", [128, 8]); n2b = B("n2b", [128, 8])
    hT_r = ring("hT", SR, [128, 8, TS], BF16)
    cacc = ring("cacc", 2, [128, TS])
    cst_all = B("cst_all", [128, 24, TS + 3], BF16); cst_t = [T("cst%d" % i) for i in range(24)]
    xsT = ring("xsT", 2, [128, 4, TS], BF16); xbc_tm = ring("xbc_tm", 2, [128, 512], BF16)
    xs_tm_r = ring("xs_tm", SR, [128, NCH, 2048], BF16)
    BT_r = ring("BT", SR, [128, 4, TS], BF16); CT_r = ring("CT", SR, [128, 4, TS], BF16); Btok_r = ring("Btok", SR, [128, NCH, 512], BF16)
    zs_r = ring("zs", SR, [128, NCH, 2048], BF16)
    gsg_r = ring("gsg", SR, [128, 2048], BF16)
    dtx = B("dtx", [128, 32]); dte_ = B("dte_", [128, 32]); dt_r = ring("dt", SR, [128, NCH, 32]); dtA_r = ring("dtA", SR, [128, NCH, 32])
    acum = B("acum", [128, 32]); ea = B("ea", [128, 32]); dte = B("dte", [128, 32]); cdb = B("cdb", [128, 32])
    lhs_all = ring("lhs_all", 2, [128, 4, 128])
    LT = ring("LT", 2, [128, 8, 128], BF16); Mh = ring("Mh", 2, [128, 8, 128], BF16)
    cbt = B("cbt", [128, 4, 128], BF16)
    xdt = ring("xdt", 2, [128, 512], BF16); xdte = ring("xdte", 2, [128, 512], BF16)
    yg = ring("yg", 2, [128, 512]); ytmp = ring("ytmp", 2, [128, 512])
    gss = B("gss", [128, 4]); yn = B("yn", [128, 2048], BF16)
    ynT = B("ynT", [128, 16, TS], BF16)
    state = B("state", [128, 2048]); state_b = B("state_b", [128, 2048], BF16)
    qkv = B("qkv", [128, 1536]); qsq = B("qsq", [128, 1280]); qss = B("qss", [128, 20])
    qr = B("qr", [128, 20, 64], BF16); rt = ring("rt", 4, [128, 20, 8])
    v1 = ring("v1", 2, [128, 4, 65], BF16)
    qT = B("qT", [64, 16, 128], BF16); kT = ring("kT", 2, [64, 4, 128], BF16)
    PT = ring("PT", 4, [128, 512], BF16)
    den = B("den", [128, 4]); att = B("att", [128, 16, 64], BF16)
    attT = B("attT", [128, 8, TS], BF16)
    mtmp = ring("mtmp", 2, [128, 512]); mergedT = B("mergedT", [128, 8, TS], BF16); mrgb = B("mrgb", [128, D], BF16)
    h2 = B("h2", [128, D]); h2b = ring("h2b", 2, [128, D], BF16); h2T = B("h2T", [128, 8, 128])
    lg = B("lg", [128, 72]); rs = ring("rs", 12, [128, 64])
    slots_f = B("slots_f", [128, 2]); slots_i = B("slots_i", [128, NCHUNK, 2], I32); gates = B("gates", [128, NCHUNK, 2])
    carry = B("carry", [128, 64]); moh = B("moh", [128, 64], BF16)
    OP('pool', 'memset', [], [state.t], state.ap, 0.0)
    OP('pool', 'memset', [], [state_b.t], state_b.ap, 0.0)
    OP('pool', 'memset', [], [carry.t], carry.ap, 0.0)
    OP('pool', 'memset', [], cst_t, cst_all.ap, 0.0)
    for i in range(2):
        OP('pool', 'memset', [], [v1[i].t], v1[i].ap, 1.0)
    t_zero = T("zero")
    ztile = B("ztile", [128, 512], BF16)
    OP('pool', 'memset', [], [ztile.t], ztile.ap, 0.0)
    t_scat = []; t_out = [T("out%d" % i) for i in range(NCHUNK)]

    def dump(name, b_, shape=None):
        if not dbg:
            return
        shp = list(b_.ap.shape)
        d_ = nc.dram_tensor("dbg_" + name, shp, b_.ap.dtype, kind="ExternalOutput").ap()
        dbg_outs[name] = T(name)
        OP('sp', 'dma_start', [b_.t], [dbg_outs[name]], out=d_, in_=b_.ap, dma='dbg')

    mhalf = B("mhalf", [128, 32])
    OP('pool', 'memset', [], [mhalf.t], mhalf.ap, -0.5)

    def rstd_from_ss(ss_ap, ss_t, n, width, tmp, out_b):
        OP('pool', 'tensor_scalar', [ss_t], [tmp.t], tmp.ap[:, 0:width], ss_ap, 1.0 / n, EPS, ALU.mult, ALU.add)
        OP('pool', 'tensor_tensor', [tmp.t, mhalf.t], [out_b.t], out=out_b.ap[:, 0:width], in0=tmp.ap[:, 0:width], in1=mhalf.ap[:, 0:width], op=ALU.pow)

    mmr = [0]

    def mmbank():
        mmr[0] += 1
        return mmr[0] % 2

    def wv_in(ncols):
        return lambda ap: ap[:, 0:8 * ncols].rearrange("p (k c) -> p k c", k=8)

    def load_in_seg(c0, ncols):
        return wload(wv_in(ncols), w_in_b[:, c0:c0 + ncols].rearrange("(k p) c -> p k c", p=128), tw_seg[c0])

    def gen_AB(sc):
            t0 = sc * TS
            pb_ = sc % SR
            xt = xt_r[pb_]; hT = hT_r[pb_]; xs_tm = xs_tm_r[pb_]; BT = BT_r[pb_]; CT = CT_r[pb_]; Btok = Btok_r[pb_]
            zs = zs_r[pb_]; gsg = gsg_r[pb_]; dt_ = dt_r[pb_]; dtA = dtA_r[pb_]
            stage[0] = 'A%d' % sc
            OP('sp', 'dma_start', [], [xt.t], out=xt.ap, in_=x_d[t0:t0 + TS, :].rearrange("(c p) d -> p c d", p=128), dma='xt%d' % pb_)
            for c in range(NCH):
                OP('act', 'activation', [xt.t], [xn.t, n1a.t], out=xn.ap, in_=xt.ap[:, c, :], func=AF.Square, accum_out=n1a.ap[:, 0:1])
                rstd_from_ss(n1a.ap[:, 0:1], n1a.t, D, 1, n1b, n1a)
                OP('dve', 'tensor_scalar', [xt.t, n1a.t], [xn.t], xn.ap, xt.ap[:, c, :], n1a.ap[:, 0:1], None, ALU.mult)
                tp = psb[2].bitcast(BF16)
                for k in range(8):
                    OP('pe', 'transpose', [xn.t, ident_b.t], [pst[2]], tp[:, k * 128:(k + 1) * 128], xn.ap[:, k * 128:(k + 1) * 128], ident_b.ap)
                OP('dve', 'tensor_tensor', [pst[2], g1T.t], [hT.t], out=hT.ap[:, :, c * 128:(c + 1) * 128],
                   in0=tp.rearrange("p (k t) -> p k t", k=8), in1=g1T.ap.unsqueeze(2).to_broadcast([128, 8, 128]), op=ALU.mult)

            yield
            stage[0] = 'Bx%d' % sc
            def conv_tile(ps_ap, ps_t, ct, out_ap, out_t):
                cs_ap = cst_all.ap[:, ct, :]
                cs_t = cst_t[ct]
                ca = cacc[ct % 2]
                eng = 'dve'
                OP('act', 'activation', [ps_t], [cs_t], out=cs_ap[:, 3:3 + TS], in_=ps_ap, func=AF.Copy)
                OP(eng, 'tensor_scalar', [cs_t, convw.t, convb.t], [ca.t], ca.ap, cs_ap[:, 3:3 + TS], convw.ap[:, ct, 3:4], convb.ap[:, ct:ct + 1], ALU.mult, ALU.add)
                for kk in (2, 1, 0):
                    OP(eng, 'scalar_tensor_tensor', [cs_t, convw.t, ca.t], [ca.t], out=ca.ap, in0=cs_ap[:, kk:kk + TS], scalar=convw.ap[:, ct, kk:kk + 1], in1=ca.ap, op0=ALU.mult, op1=ALU.add)
                OP('act', 'activation', [cs_t], [cs_t], out=cs_ap[:, 0:3], in_=cs_ap[:, TS:TS + 3], func=AF.Copy)
                OP('act', 'activation', [ca.t], [out_t], out=out_ap, in_=ca.ap, func=AF.Silu)

            for seg in range(6):
                if seg > 0:
                    yield
                stage[0] = 'Bx%d' % sc
                wb = load_in_seg(C_XS + seg * 512, 512)
                wv = wv_in(512)(wb.ap)
                bk = mmbank()
                for k in range(8):
                    OP('pe', 'matmul', [wb.t, hT.t], [pst[bk]], psb[bk], lhsT=hT.ap[:, k, 0:128], rhs=wv[:, k, :], start=(k == 0), stop=(k == 7))
                xtm = xbc_tm[seg % 2]
                OP('act', 'activation', [pst[bk]], [xtm.t], out=xtm.ap, in_=psb[bk], func=AF.Copy)
                bq = mmbank()
                tq = psb[bq].bitcast(BF16)
                for j in range(4):
                    OP('pe', 'transpose', [xtm.t, ident_b.t], [pst[bq]], tq[:, j * 128:(j + 1) * 128], xtm.ap[:, j * 128:(j + 1) * 128], ident_b.ap)
                for j in range(4):
                    ct = seg * 4 + j
                    src = tq[:, j * 128:(j + 1) * 128]
                    if seg < 4:
                        xb = xsT[seg % 2]
                        conv_tile(src, pst[bq], ct, xb.ap[:, j, :], xb.t)
                    elif seg == 4:
                        conv_tile(src, pst[bq], ct, BT.ap[:, j, :], BT.t)
                    else:
                        conv_tile(src, pst[bq], ct, CT.ap[:, j, :], CT.t)
                tp = psb[2].bitcast(BF16)
                if seg < 4:
                    xb = xsT[seg % 2]
                    for c in range(NCH):
                        for j in range(4):
                            OP('pe', 'transpose', [xb.t, ident_b.t], [pst[2]], tp[:, j * 128:(j + 1) * 128], xb.ap[:, j, c * 128:(c + 1) * 128], ident_b.ap)
                        OP('dve', 'tensor_copy', [pst[2]], [xs_tm.t], out=xs_tm.ap[:, c, seg * 512:(seg + 1) * 512], in_=tp[:, 0:512])
                elif seg == 4:
                    for c in range(NCH):
                        for j in range(4):
                            OP('pe', 'transpose', [BT.t, ident_b.t], [pst[2]], tp[:, j * 128:(j + 1) * 128], BT.ap[:, j, c * 128:(c + 1) * 128], ident_b.ap)
                        OP('dve', 'tensor_copy', [pst[2]], [Btok.t], out=Btok.ap[:, c, :], in_=tp[:, 0:512])

            yield
            stage[0] = 'Bd%d' % sc
            wb = load_in_seg(C_DT, 32)
            wv = wv_in(32)(wb.ap)
            for c in range(NCH):
                bk = mmbank()
                for k in range(8):
                    OP('pe', 'matmul', [wb.t, hT.t], [pst[bk]], psb[bk][:, 0:32], lhsT=hT.ap[:, k, c * 128:(c + 1) * 128], rhs=wv[:, k, :], start=(k == 0), stop=(k == 7))
                OP('dve', 'tensor_tensor', [pst[bk], rowc.t], [dtx.t], out=dtx.ap, in0=psb[bk][:, 0:32], in1=rc[:, R_DTB:R_DTB + 32], op=ALU.add)
                OP('act', 'activation', [dtx.t], [dte_.t], out=dte_.ap, in_=dtx.ap, func=AF.Exp)
                OP('act', 'activation', [dte_.t], [dt_.t], out=dt_.ap[:, c, :], in_=dte_.ap, func=AF.Ln, bias=1.0)
                OP('dve', 'tensor_tensor', [dt_.t, a_neg.t], [dtA.t], out=dtA.ap[:, c, :], in0=dt_.ap[:, c, :], in1=a_neg.ap, op=ALU.mult)

            yield
            stage[0] = 'Bz%d' % sc
            for seg in range(4):
                if seg > 0:
                    yield
                stage[0] = 'Bz%d' % sc
                wb = load_in_seg(C_Z + seg * 512, 512)
                wv = wv_in(512)(wb.ap)
                for c in range(NCH):
                    bk = mmbank()
                    for k in range(8):
                        OP('pe', 'matmul', [wb.t, hT.t], [pst[bk]], psb[bk], lhsT=hT.ap[:, k, c * 128:(c + 1) * 128], rhs=wv[:, k, :], start=(k == 0), stop=(k == 7))
                    OP('act', 'activation', [pst[bk]], [zs.t], out=zs.ap[:, c, seg * 512:(seg + 1) * 512], in_=psb[bk], func=AF.Silu)

            yield
            stage[0] = 'Bg%d' % sc
            for seg in range(4):
                stage[0] = 'Bg%d' % sc
                wb = load_in_seg(C_GS + seg * 512, 512)
                wv = wv_in(512)(wb.ap)
                for c in range(NCH):
                    bk = mmbank()
                    for k in range(8):
                        OP('pe', 'matmul', [wb.t, hT.t], [pst[bk]], psb[bk], lhsT=hT.ap[:, k, c * 128:(c + 1) * 128], rhs=wv[:, k, :], start=(k == 0), stop=(k == 7))
                    OP('act', 'activation', [pst[bk]], [gsg.t], out=gsg.ap[:, seg * 512:(seg + 1) * 512], in_=psb[bk], func=AF.Tanh, scale=0.5)

            yield

    def gen_CF(sc):
            t0 = sc * TS
            pb_ = sc % SR
            xt = xt_r[pb_]; hT = hT_r[pb_]; xs_tm = xs_tm_r[pb_]; BT = BT_r[pb_]; CT = CT_r[pb_]; Btok = Btok_r[pb_]
            zs = zs_r[pb_]; gsg = gsg_r[pb_]; dt_ = dt_r[pb_]; dtA = dtA_r[pb_]
            stage[0] = 'C%d' % sc
            for c in range(NCH):
                tsl = slice(c * 128, (c + 1) * 128)
                dA = dtA.ap[:, c, :]
                OP('pe', 'matmul', [maskle.t, dtA.t], [pst[4]], psb[4][:, 0:32], lhsT=maskle.ap, rhs=dA, start=True, stop=True)
                OP('pe', 'matmul', [ones_f.t, dtA.t], [pst[4]], psb[4][:, 32:64], lhsT=ones_f.ap, rhs=dA, start=True, stop=True)
                OP('act', 'activation', [pst[4]], [ea.t], out=ea.ap, in_=psb[4][:, 0:32], func=AF.Exp)
                OP('act', 'activation', [pst[4]], [cdb.t], out=cdb.ap, in_=psb[4][:, 32:64], func=AF.Exp)
                OP('dve', 'tensor_copy', [pst[4]], [acum.t], out=acum.ap, in_=psb[4][:, 0:32])
                OP('dve', 'tensor_tensor', [pst[4], acum.t], [dte.t], out=dte.ap, in0=psb[4][:, 32:64], in1=acum.ap, op=ALU.subtract)
                OP('act', 'activation', [dte.t], [dte.t], out=dte.ap, in_=dte.ap, func=AF.Exp)
                for g in range(4):
                    OP('pe', 'matmul', [BT.t, CT.t], [pst[7]], psb[7][:, g * 128:(g + 1) * 128], lhsT=BT.ap[:, g, tsl], rhs=CT.ap[:, g, tsl], start=True, stop=True)
                OP('dve', 'tensor_tensor', [pst[7], maskle.t], [cbt.t], out=cbt.ap, in0=psb[7].rearrange("p (g l) -> p g l", g=4),
                   in1=maskle.ap.unsqueeze(1).to_broadcast([128, 4, 128]), op=ALU.mult)
                for g in range(4):
                    yield
                    stage[0] = 'C%d' % sc
                    r = g % 2
                    hs = slice(g * 8, (g + 1) * 8)
                    fs = slice(g * 512, (g + 1) * 512)
                    for hb in range(2):
                        h0 = g * 8 + hb * 4
                        la = lhs_all[hb]
                        OP('pool', 'tensor_tensor', [maskgt.t, dtA.t], [la.t], out=la.ap,
                           in0=maskgt.ap.unsqueeze(1).to_broadcast([128, 4, 128]), in1=dA[:, h0:h0 + 4].unsqueeze(2).to_broadcast([128, 4, 128]), op=ALU.mult)
                        bk = 3 + hb
                        for hh in range(4):
                            OP('pe', 'matmul', [la.t, maskle.t], [pst[bk]], psb[bk][:, hh * 128:(hh + 1) * 128], lhsT=la.ap[:, hh, :], rhs=maskle.ap, start=True, stop=True)
                        OP('act', 'activation', [pst[bk]], [LT[r].t], out=LT[r].ap[:, hb * 4:(hb + 1) * 4, :], in_=psb[bk].rearrange("p (h l) -> p h l", h=4), func=AF.Exp)
                    OP('dve', 'tensor_tensor', [LT[r].t, cbt.t], [Mh[r].t], out=Mh[r].ap, in0=LT[r].ap,
                       in1=cbt.ap[:, g, :].unsqueeze(1).to_broadcast([128, 8, 128]), op=ALU.mult)
                    xs_g = xs_tm.ap[:, c, fs].rearrange("p (h d) -> p h d", h=8)
                    OP('pool', 'tensor_tensor', [xs_tm.t, dt_.t], [xdt[r].t], out=xdt[r].ap.rearrange("p (h d) -> p h d", h=8), in0=xs_g,
                       in1=dt_.ap[:, c, hs].unsqueeze(2).to_broadcast([128, 8, 64]), op=ALU.mult)
                    OP('pool', 'tensor_tensor', [xdt[r].t, dte.t], [xdte[r].t], out=xdte[r].ap.rearrange("p (h d) -> p h d", h=8),
                       in0=xdt[r].ap.rearrange("p (h d) -> p h d", h=8), in1=dte.ap[:, hs].unsqueeze(2).to_broadcast([128, 8, 64]), op=ALU.mult)
                    for hh in range(8):
                        OP('pe', 'matmul', [Mh[r].t, xdt[r].t], [pst[5]], psb[5][:, hh * 64:(hh + 1) * 64], lhsT=Mh[r].ap[:, hh, :], rhs=xdt[r].ap[:, hh * 64:(hh + 1) * 64], start=True, stop=True)
                    OP('pe', 'matmul', [CT.t, state_b.t], [pst[6]], psb[6], lhsT=CT.ap[:, g, tsl], rhs=state_b.ap[:, fs], start=True, stop=True)
                    OP('pe', 'matmul', [Btok.t, xdte[r].t], [pst[7]], psb[7], lhsT=Btok.ap[:, c, g * 128:(g + 1) * 128], rhs=xdte[r].ap, start=True, stop=True)
                    y_ = yg[r]; yt = ytmp[r]
                    v3 = lambda ap: ap.rearrange("p (h d) -> p h d", h=8)
                    OP('dve', 'tensor_tensor', [pst[6], ea.t], [yt.t], out=v3(yt.ap), in0=v3(psb[6]), in1=ea.ap[:, hs].unsqueeze(2).to_broadcast([128, 8, 64]), op=ALU.mult)
                    OP('dve', 'tensor_tensor', [pst[5], yt.t], [y_.t], out=y_.ap, in0=psb[5], in1=yt.ap, op=ALU.add)
                    OP('pool', 'tensor_tensor', [xs_tm.t, rowc.t], [yt.t], out=v3(yt.ap), in0=xs_g, in1=rc[:, R_DSK + g * 8:R_DSK + g * 8 + 8].unsqueeze(2).to_broadcast([128, 8, 64]), op=ALU.mult)
                    OP('dve', 'tensor_tensor', [y_.t, yt.t], [y_.t], out=y_.ap, in0=y_.ap, in1=yt.ap, op=ALU.add)
                    OP('dve', 'tensor_tensor', [y_.t, zs.t], [y_.t], out=y_.ap, in0=y_.ap, in1=zs.ap[:, c, fs], op=ALU.mult)
                    OP('act', 'activation', [y_.t], [yt.t, gss.t], out=yt.ap, in_=y_.ap, func=AF.Square, accum_out=gss.ap[:, g:g + 1])
                    rstd_from_ss(gss.ap[:, g:g + 1], gss.t, 512, 1, st2, st1)
                    OP('dve', 'tensor_scalar', [y_.t, st1.t], [yn.t], yn.ap[:, fs], y_.ap, st1.ap[:, 0:1], None, ALU.mult)
                    OP('dve', 'tensor_tensor', [state.t, cdb.t], [state.t], out=v3(state.ap[:, fs]), in0=v3(state.ap[:, fs]), in1=cdb.ap[:, hs].unsqueeze(2).to_broadcast([128, 8, 64]), op=ALU.mult)
                    OP('dve', 'tensor_tensor', [state.t, pst[7]], [state.t], out=state.ap[:, fs], in0=state.ap[:, fs], in1=psb[7], op=ALU.add)
                    OP('act', 'activation', [state.t], [state_b.t], out=state_b.ap[:, fs], in_=state.ap[:, fs], func=AF.Copy)
                tp = psb[2].bitcast(BF16)
                for half in range(2):
                    for j in range(8):
                        ctile = half * 8 + j
                        OP('pe', 'transpose', [yn.t, ident_b.t], [pst[2]], tp[:, j * 128:(j + 1) * 128], yn.ap[:, ctile * 128:(ctile + 1) * 128], ident_b.ap)
                    OP('dve', 'tensor_tensor', [pst[2], gssdT.t], [ynT.t], out=ynT.ap[:, half * 8:(half + 1) * 8, tsl], in0=tp.rearrange("p (k t) -> p k t", k=8),
                       in1=gssdT.ap[:, half * 8:(half + 1) * 8].unsqueeze(2).to_broadcast([128, 8, 128]), op=ALU.mult)

            yield
            stage[0] = 'D%d' % sc
            wq = [load_in_seg(C_Q, 512), load_in_seg(C_Q + 512, 512), load_in_seg(C_K, 512)]
            for c in range(NCH):
                ci = sc * NCH + c
                tsl = slice(c * 128, (c + 1) * 128)
                for s3 in range(3):
                    bk = mmbank()
                    wv = wv_in(512)(wq[s3].ap)
                    for k in range(8):
                        OP('pe', 'matmul', [wq[s3].t, hT.t], [pst[bk]], psb[bk], lhsT=hT.ap[:, k, tsl], rhs=wv[:, k, :], start=(k == 0), stop=(k == 7))
                    OP('act', 'activation', [pst[bk]], [qkv.t], out=qkv.ap[:, s3 * 512:(s3 + 1) * 512], in_=psb[bk], func=AF.Copy)
                cur = ci % 2; prv = 1 - cur
                OP('pool', 'tensor_copy', [qkv.t], [v1[cur].t], out=v1[cur].ap[:, :, 0:64], in_=qkv.ap[:, 1280:1536].rearrange("p (h d) -> p h d", h=4))
                OP('act', 'activation', [qkv.t], [qsq.t], out=qsq.ap, in_=qkv.ap[:, 0:1280], func=AF.Square)
                OP('dve', 'tensor_reduce', [qsq.t], [qss.t], out=qss.ap, in_=qsq.ap.rearrange("p (h d) -> p h d", h=20), axis=AX.X, op=ALU.add)
                OP('pool', 'tensor_scalar', [qss.t], [qss.t], qss.ap, qss.ap, 1.0 / 64, EPS, ALU.mult, ALU.add)
                OP('pool', 'tensor_tensor', [qss.t, mhalf.t], [qss.t], out=qss.ap, in0=qss.ap, in1=mhalf.ap[:, 0:20], op=ALU.pow)
                q3 = qsq.ap.rearrange("p (h d) -> p h d", h=20)
                OP('dve', 'tensor_tensor', [qkv.t, qss.t], [qsq.t], out=q3, in0=qkv.ap[:, 0:1280].rearrange("p (h d) -> p h d", h=20),
                   in1=qss.ap.unsqueeze(2).to_broadcast([128, 20, 64]), op=ALU.mult)
                OP('dve', 'tensor_tensor', [qsq.t, rowc.t], [qsq.t], out=qsq.ap, in0=qsq.ap, in1=rc[:, R_QKG:R_QKG + 1280], op=ALU.mult)
                cosb = cs_all.ap[:, ci, 0:8].unsqueeze(1).to_broadcast([128, 20, 8])
                sinb = cs_all.ap[:, ci, 8:16].unsqueeze(1).to_broadcast([128, 20, 8])
                t1 = q3[:, :, 0:8]; t2 = q3[:, :, 8:16]
                OP('pool', 'tensor_copy', [qsq.t], [qr.t], out=qr.ap[:, :, 16:64], in_=q3[:, :, 16:64])
                OP('dve', 'tensor_tensor', [qsq.t, cs_all.t], [rt[0].t], out=rt[0].ap, in0=t1, in1=cosb, op=ALU.mult)
                OP('dve', 'tensor_tensor', [qsq.t, cs_all.t], [rt[1].t], out=rt[1].ap, in0=t2, in1=sinb, op=ALU.mult)
                OP('dve', 'tensor_tensor', [qsq.t, cs_all.t], [rt[2].t], out=rt[2].ap, in0=t2, in1=cosb, op=ALU.mult)
                OP('dve', 'tensor_tensor', [qsq.t, cs_all.t], [rt[3].t], out=rt[3].ap, in0=t1, in1=sinb, op=ALU.mult)
                OP('dve', 'tensor_tensor', [rt[0].t, rt[1].t], [qr.t], out=qr.ap[:, :, 0:8], in0=rt[0].ap, in1=rt[1].ap, op=ALU.subtract)
                OP('dve', 'tensor_tensor', [rt[2].t, rt[3].t], [qr.t], out=qr.ap[:, :, 8:16], in0=rt[2].ap, in1=rt[3].ap, op=ALU.add)
                tp = psb[2].bitcast(BF16)
                for half in range(2):
                    for j in range(8):
                        OP('pe', 'transpose', [qr.t, ident_b.t], [pst[2]], tp[0:64, j * 128:(j + 1) * 128], qr.ap[:, half * 8 + j, :], ident_b.ap)
                    OP('dve', 'tensor_copy', [pst[2]], [qT.t], out=qT.ap[:, half * 8:(half + 1) * 8, :], in_=tp[0:64, :].rearrange("p (h t) -> p h t", h=8))
                for j in range(4):
                    OP('pe', 'transpose', [qr.t, ident_b.t], [pst[2]], tp[0:64, j * 128:(j + 1) * 128], qr.ap[:, 16 + j, :], ident_b.ap)
                OP('dve', 'tensor_copy', [pst[2]], [kT[cur].t], out=kT[cur].ap, in_=tp[0:64, 0:512].rearrange("p (h t) -> p h t", h=4))
                for g in range(4):
                    yield
                    stage[0] = 'D%d' % sc
                    rhs_q = qT.ap[:, g * 4:(g + 1) * 4, :]
                    blocks = [(cur, negcur, 0)] + ([(prv, negprev, 1)] if ci > 0 else [])
                    pts = []
                    for (bi, negm, w_) in blocks:
                        bk = 3 + w_
                        OP('pe', 'matmul', [kT[bi].t, qT.t], [pst[bk]], psb[bk], lhsT=kT[bi].ap[:, g, :], rhs=rhs_q, start=True, stop=False)
                        OP('pe', 'matmul', [ident_b.t, negm.t], [pst[bk]], psb[bk], lhsT=ident_b.ap, rhs=negm.ap, start=False, stop=True)
                        pt = PT[(g % 2) * 2 + w_]
                        OP('act', 'activation', [pst[bk]], [pt.t], out=pt.ap, in_=psb[bk], func=AF.Exp, scale=0.125)
                        pts.append((pt, bi))
                    pv = psb[5][:, 0:260].rearrange("p (h d) -> p h d", h=4)
                    for hh in range(4):
                        for i_, (pt, bi) in enumerate(pts):
                            OP('pe', 'matmul', [pt.t, v1[bi].t], [pst[5]], pv[:, hh, :], lhsT=pt.ap[:, hh * 128:(hh + 1) * 128], rhs=v1[bi].ap[:, g, :], start=(i_ == 0), stop=(i_ == len(pts) - 1))
                    OP('dve', 'tensor_tensor', [pst[5], esink.t], [den.t], out=den.ap, in0=pv[:, :, 64], in1=esink.ap[:, g * 4:(g + 1) * 4], op=ALU.add)
                    OP('dve', 'reciprocal', [den.t], [den.t], out=den.ap, in_=den.ap)
                    OP('dve', 'tensor_tensor', [pst[5], den.t], [att.t], out=att.ap[:, g * 4:(g + 1) * 4, :], in0=pv[:, :, 0:64], in1=den.ap.unsqueeze(2).to_broadcast([128, 4, 64]), op=ALU.mult)
                tp = psb[2].bitcast(BF16)
                attf = att.ap.rearrange("p h d -> p (h d)")
                for j in range(8):
                    OP('pe', 'transpose', [att.t, ident_b.t], [pst[2]], tp[:, j * 128:(j + 1) * 128], attf[:, j * 128:(j + 1) * 128], ident_b.ap)
                OP('dve', 'tensor_copy', [pst[2]], [attT.t], out=attT.ap[:, :, tsl], in_=tp.rearrange("p (k t) -> p k t", k=8))

            yield
            stage[0] = 'E%d' % sc
            assert NCH == 1
            for ch in range(2):
                bs = mmbank()
                for kh in range(2):
                    wb = wload(wv_in(512), w_so_b[kh * 1024:(kh + 1) * 1024, ch * 512:(ch + 1) * 512].rearrange("(k p) c -> p k c", p=128), tw_so)
                    wv = wv_in(512)(wb.ap)
                    for k in range(8):
                        OP('pe', 'matmul', [wb.t, ynT.t], [pst[bs]], psb[bs], lhsT=ynT.ap[:, kh * 8 + k, :], rhs=wv[:, k, :], start=(kh == 0 and k == 0), stop=(kh == 1 and k == 7))
                OP('dve', 'scalar_tensor_tensor', [gsg.t, pst[bs]], [mrg.t], out=mrg.ap[:, ch * 512:(ch + 1) * 512], in0=gsg.ap[:, ch * 512:(ch + 1) * 512], scalar=1.0, in1=psb[bs], op0=ALU.add, op1=ALU.mult)
            for ch in range(2):
                wb = wload(wv_in(512), w_ao_b[:, ch * 512:(ch + 1) * 512].rearrange("(k p) c -> p k c", p=128), tw_ao)
                wv = wv_in(512)(wb.ap)
                bk = mmbank()
                for k in range(8):
                    OP('pe', 'matmul', [wb.t, attT.t], [pst[bk]], psb[bk], lhsT=attT.ap[:, k, :], rhs=wv[:, k, :], start=(k == 0), stop=(k == 7))
                mt = mtmp[ch]
                OP('dve', 'scalar_tensor_tensor', [gsg.t, pst[bk]], [mt.t], out=mt.ap, in0=gsg.ap[:, 1024 + ch * 512:1024 + (ch + 1) * 512], scalar=1.0, in1=psb[bk], op0=ALU.add, op1=ALU.mult)
                OP('pool', 'tensor_tensor', [mt.t, mrg.t], [mrgb.t], out=mrgb.ap[:, ch * 512:(ch + 1) * 512], in0=mt.ap, in1=mrg.ap[:, ch * 512:(ch + 1) * 512], op=ALU.add)
            tp = psb[2].bitcast(BF16)
            for j in range(8):
                OP('pe', 'transpose', [mrgb.t, ident_b.t], [pst[2]], tp[:, j * 128:(j + 1) * 128], mrgb.ap[:, j * 128:(j + 1) * 128], ident_b.ap)
            OP('act', 'activation', [pst[2]], [mergedT.t], out=mergedT.ap, in_=tp.rearrange("p (k t) -> p k t", k=8), func=AF.Copy)
            yield
            stage[0] = 'F%d' % sc
            wo = [wload(wv_in(512), w_o_b[:, seg * 512:(seg + 1) * 512].rearrange("(k p) c -> p k c", p=128), tw_o) for seg in range(2)]
            for c in range(NCH):
                ci = sc * NCH + c
                tsl = slice(c * 128, (c + 1) * 128)
                for seg in range(2):
                    wv = wv_in(512)(wo[seg].ap)
                    bk = mmbank()
                    for k in range(8):
                        OP('pe', 'matmul', [wo[seg].t, mergedT.t], [pst[bk]], psb[bk], lhsT=mergedT.ap[:, k, tsl], rhs=wv[:, k, :], start=(k == 0), stop=(k == 7))
                    OP('dve', 'scalar_tensor_tensor', [pst[bk], xt.t], [xt.t], out=xt.ap[:, c, seg * 512:(seg + 1) * 512], in0=psb[bk], scalar=0.5, in1=xt.ap[:, c, seg * 512:(seg + 1) * 512], op0=ALU.mult, op1=ALU.add)
                OP('sp', 'dma_start', [xt.t], [t_out[ci]], out=out_d[ci * 128:(ci + 1) * 128, :], in_=xt.ap[:, c, :], dma='x2st%d' % (ci % 2))
                hb_ = h2b[ci % 2]
                OP('act', 'activation', [xt.t], [hb_.t, n2a.t], out=hb_.ap, in_=xt.ap[:, c, :], func=AF.Square, accum_out=n2a.ap[:, 0:1])
                rstd_from_ss(n2a.ap[:, 0:1], n2a.t, D, 1, n2b, n2a)
                OP('dve', 'scalar_tensor_tensor', [xt.t, n2a.t, rowc.t], [h2.t], out=h2.ap, in0=xt.ap[:, c, :], scalar=n2a.ap[:, 0:1], in1=rc[:, R_G2:R_G2 + D], op0=ALU.mult, op1=ALU.mult)
                hb_ = h2b[ci % 2]
                OP('pool', 'tensor_copy', [h2.t], [hb_.t], out=hb_.ap, in_=h2.ap)
                for half in range(2):
                    for j in range(4):
                        k = half * 4 + j
                        OP('pe', 'transpose', [h2.t, ident_f.t], [pst[2]], psb[2][:, j * 128:(j + 1) * 128], h2.ap[:, k * 128:(k + 1) * 128], ident_f.ap)
                    OP('dve', 'tensor_copy', [pst[2]], [h2T.t], out=h2T.ap[:, half * 4:(half + 1) * 4, :], in_=psb[2].rearrange("p (k t) -> p k t", k=4))
                bk = mmbank()
                for k in range(8):
                    OP('pe', 'matmul', [h2T.t, wr.t], [pst[bk]], psb[bk][:, 0:72], lhsT=h2T.ap[:, k, :], rhs=wr.ap[:, k, :], start=(k == 0), stop=(k == 7))
                OP('dve', 'tensor_tensor', [pst[bk], rowc.t], [lg.t], out=lg.ap, in0=psb[bk][:, 0:72], in1=rc[:, R_RB:R_RB + 72], op=ALU.add)
                gl = lg.ap[:, 0:8]
                gmax, gsh, gex, gsum, goh, esel8, e1, oh1, e2, m2, oh2, tmpa = rs
                OP('dve', 'tensor_reduce', [lg.t], [gmax.t], out=gmax.ap[:, 0:1], in_=gl, axis=AX.X, op=ALU.max)
                OP('dve', 'tensor_scalar', [lg.t, gmax.t], [goh.t], goh.ap[:, 0:8], gl, gmax.ap[:, 0:1], None, ALU.is_equal)
                OP('dve', 'tensor_scalar', [lg.t, gmax.t], [gsh.t], gsh.ap[:, 0:8], gl, gmax.ap[:, 0:1], None, ALU.subtract)
                OP('act', 'activation', [gsh.t], [gex.t, gsum.t], out=gex.ap[:, 0:8], in_=gsh.ap[:, 0:8], func=AF.Exp, accum_out=gsum.ap[:, 0:1])
                OP('dve', 'reciprocal', [gsum.t], [gsum.t], out=gsum.ap[:, 0:1], in_=gsum.ap[:, 0:1])
                el3 = lg.ap[:, 8:72].rearrange("p (g e) -> p g e", g=8)
                OP('dve', 'tensor_tensor', [lg.t, goh.t], [esel8.t], out=esel8.ap.rearrange("p (g e) -> p g e", g=8), in0=el3, in1=goh.ap[:, 0:8].unsqueeze(2).to_broadcast([128, 8, 8]), op=ALU.mult)
                OP('dve', 'tensor_reduce', [esel8.t], [e1.t], out=e1.ap[:, 0:8], in_=esel8.ap.rearrange("p (g e) -> p e g", g=8), axis=AX.X, op=ALU.add)
                OP('dve', 'tensor_reduce', [e1.t], [gmax.t], out=gmax.ap[:, 1:2], in_=e1.ap[:, 0:8], axis=AX.X, op=ALU.max)
                OP('dve', 'tensor_scalar', [e1.t, gmax.t], [oh1.t], oh1.ap[:, 0:8], e1.ap[:, 0:8], gmax.ap[:, 1:2], None, ALU.is_equal)
                OP('dve', 'scalar_tensor_tensor', [oh1.t, e1.t], [e2.t], out=e2.ap[:, 0:8], in0=oh1.ap[:, 0:8], scalar=-1e30, in1=e1.ap[:, 0:8], op0=ALU.mult, op1=ALU.add)
                OP('dve', 'tensor_reduce', [e2.t], [gmax.t], out=gmax.ap[:, 2:3], in_=e2.ap[:, 0:8], axis=AX.X, op=ALU.max)
                OP('dve', 'tensor_scalar', [e2.t, gmax.t], [oh2.t], oh2.ap[:, 0:8], e2.ap[:, 0:8], gmax.ap[:, 2:3], None, ALU.is_equal)
                OP('dve', 'tensor_tensor', [gmax.t], [m2.t], out=m2.ap[:, 0:1], in0=gmax.ap[:, 2:3], in1=gmax.ap[:, 1:2], op=ALU.subtract)
                OP('act', 'activation', [m2.t], [m2.t], out=m2.ap[:, 1:2], in_=m2.ap[:, 0:1], func=AF.Exp)
                OP('dve', 'tensor_scalar', [m2.t], [m2.t], m2.ap[:, 2:3], m2.ap[:, 1:2], 1.0, None, ALU.add)
                OP('dve', 'reciprocal', [m2.t], [m2.t], out=m2.ap[:, 3:4], in_=m2.ap[:, 2:3])
                OP('dve', 'tensor_tensor', [m2.t], [m2.t], out=m2.ap[:, 4:5], in0=m2.ap[:, 1:2], in1=m2.ap[:, 3:4], op=ALU.mult)
                OP('dve', 'tensor_scalar', [m2.t, gsum.t], [gates.t], gates.ap[:, ci, :], m2.ap[:, 3:5], gsum.ap[:, 0:1], None, ALU.mult)
                f1 = tmpa; f2 = gsh
                OP('dve', 'tensor_tensor', [goh.t, oh1.t], [f1.t], out=f1.ap.rearrange("p (g e) -> p g e", g=8), in0=goh.ap[:, 0:8].unsqueeze(2).to_broadcast([128, 8, 8]),
                   in1=oh1.ap[:, 0:8].unsqueeze(1).to_broadcast([128, 8, 8]), op=ALU.mult)
                OP('dve', 'tensor_tensor', [goh.t, oh2.t], [f2.t], out=f2.ap.rearrange("p (g e) -> p g e", g=8), in0=goh.ap[:, 0:8].unsqueeze(2).to_broadcast([128, 8, 8]),
                   in1=oh2.ap[:, 0:8].unsqueeze(1).to_broadcast([128, 8, 8]), op=ALU.mult)
                OP('dve', 'tensor_tensor', [f1.t, f2.t], [moh.t], out=moh.ap, in0=f1.ap, in1=f2.ap, op=ALU.add)
                OP('pe', 'matmul', [ustr_b.t, moh.t], [pst[6]], psb[6][:, 0:64], lhsT=ustr_b.ap, rhs=moh.ap, start=True, stop=True)
                OP('pe', 'matmul', [ones_b.t, moh.t], [pst[6]], psb[6][:, 64:128], lhsT=ones_b.ap, rhs=moh.ap, start=True, stop=True)
                rk = esel8
                OP('dve', 'tensor_tensor', [pst[6], carry.t], [rk.t], out=rk.ap, in0=psb[6][:, 0:64], in1=carry.ap, op=ALU.add)
                OP('dve', 'tensor_tensor', [pst[6], carry.t], [carry.t], out=carry.ap, in0=psb[6][:, 64:128], in1=carry.ap, op=ALU.add)
                OP('dve', 'tensor_scalar', [rk.t], [rk.t], rk.ap, rk.ap, float(CAP - 1), None, ALU.min)
                OP('dve', 'tensor_tensor', [rk.t, ebase_b.t], [rk.t], out=rk.ap, in0=rk.ap, in1=ebase_b.ap, op=ALU.add)
                for kk, ff in enumerate((f1, f2)):
                    OP('dve', 'tensor_tensor', [rk.t, ff.t], [ff.t], out=ff.ap, in0=rk.ap, in1=ff.ap, op=ALU.mult)
                    OP('dve', 'tensor_reduce', [ff.t], [slots_f.t], out=slots_f.ap[:, kk:kk + 1], in_=ff.ap, axis=AX.X, op=ALU.add)
                OP('dve', 'tensor_copy', [slots_f.t], [slots_i.t], out=slots_i.ap[:, ci, :], in_=slots_f.ap)
                for kk in range(2):
                    t_scat.append(T("scat"))
                    OP('pool', 'indirect_dma_start', [hb_.t, slots_i.t, t_zero], [t_scat[-1]], out=xs_scr, out_offset=bass.IndirectOffsetOnAxis(ap=slots_i.ap[:, ci, kk:kk + 1], axis=0),
                       in_=hb_.ap, in_offset=None, dma='scat%d' % (ci % 2))
            if dbg and sc == 0:
                dump("hT", hT); dump("xs_tm", xs_tm); dump("zs", zs); dump("yn", yn); dump("ynT", ynT); dump("attT", attT)
                dump("mergedT", mergedT); dump("h2", h2); dump("lg", lg); dump("dt", dt_); dump("BT", BT); dump("gsg", gsg); dump("att", att)
                dump("state", state)


            yield

    def drive(g1, g2):
        gs = [g for g in (g1, g2) if g is not None]
        while gs:
            for g in list(gs):
                try:
                    next(g)
                except StopIteration:
                    gs.remove(g)

    drive(gen_AB(0), None)
    for i in range(NSLOT // 512):
        OP('act', 'dma_start', [ztile.t, gsg_r[0].t], [t_zero], out=xs_scr[i * 512:(i + 1) * 512, :].rearrange("(p j) (a d) -> p (j a) d", p=128, a=2),
           in_=ztile.ap.unsqueeze(1).to_broadcast([128, 8, 512]), dma='zero')
    for sc in range(nsc):
        drive(gen_CF(sc), gen_AB(sc + 1) if sc + 1 < nsc else None)

    if dbg:
        dump("slots_i", slots_i); dump("gates", gates); dump("carry", carry)

    stage[0] = 'P2'
    assert NCH == 1
    wgb = [V(wbuf[i].ap, wbuf[i].t, 0, 2048, [128, 8, 256], BF16) for i in range(2)]
    wub = [V(wbuf[i].ap, wbuf[i].t, 2048, 4096, [128, 8, 256], BF16) for i in range(2)]
    wdb = [V(wbuf[2].ap, wbuf[2].t, i * 2048, (i + 1) * 2048, [128, 2, D], BF16) for i in range(2)]
    xg = [V(state.ap, state.t, i * 1024, (i + 1) * 1024, [128, 2, D], BF16) for i in range(2)]
    xgT = [V(state_b.ap, state_b.t, 0, 2048, [128, 8, CAP], BF16), V(yn.ap, yn.t, 0, 2048, [128, 8, CAP], BF16)]
    ysb = [V(xt_r[0].ap[:, 0, :], xt_r[0].t, 0, D // 2, [128, D], BF16), V(h2.ap, h2.t, 0, D // 2, [128, D], BF16)]
    sg = [V(qkv.ap, qkv.t, i * 256, (i + 1) * 256, [128, CAP], F32) for i in range(2)]
    hidT = [V(qkv.ap, qkv.t, 512 + i * 256, 768 + i * 256, [128, 2, CAP], BF16) for i in range(2)]
    t_ys = []
    for e in range(nexp):
        r = e % 2
        OP('pool', 'dma_start', [], [wgb[r].t], out=wgb[r].ap, in_=wg_d[e].rearrange("(k p) f -> p k f", p=128), dma='wg%d' % r)
        OP('pool', 'dma_start', [], [wub[r].t], out=wub[r].ap, in_=wu_d[e].rearrange("(k p) f -> p k f", p=128), dma='wu%d' % r)
        OP('pool', 'dma_start', [], [wdb[r].t], out=wdb[r].ap, in_=wd_d[e].rearrange("(k p) f -> p k f", p=128), dma='wd%d' % r)
        OP('sp', 'dma_start', t_scat, [xg[r].t], out=xg[r].ap, in_=xs_scr[e * CAP:(e + 1) * CAP, :].rearrange("(s p) d -> p s d", p=128), dma='xg%d' % r)
        tp = psb[2].bitcast(BF16)
        for s_ in range(2):
            for k in range(8):
                OP('pe', 'transpose', [xg[r].t, ident_b.t], [pst[2]], tp[:, k * 128:(k + 1) * 128], xg[r].ap[:, s_, k * 128:(k + 1) * 128], ident_b.ap)
            OP('dve', 'tensor_copy', [pst[2]], [xgT[r].t], out=xgT[r].ap[:, :, s_ * 128:(s_ + 1) * 128], in_=tp.rearrange("p (k t) -> p k t", k=8))
        for fc in range(2):
            for k in range(8):
                OP('pe', 'matmul', [wgb[r].t, xgT[r].t], [pst[0]], psb[0][:, 0:CAP], lhsT=wgb[r].ap[:, k, fc * 128:(fc + 1) * 128], rhs=xgT[r].ap[:, k, :], start=(k == 0), stop=(k == 7))
            for k in range(8):
                OP('pe', 'matmul', [wub[r].t, xgT[r].t], [pst[1]], psb[1][:, 0:CAP], lhsT=wub[r].ap[:, k, fc * 128:(fc + 1) * 128], rhs=xgT[r].ap[:, k, :], start=(k == 0), stop=(k == 7))
            OP('act', 'activation', [pst[0]], [sg[fc].t], out=sg[fc].ap, in_=psb[0][:, 0:CAP], func=AF.Silu)
            OP('dve', 'tensor_tensor', [sg[fc].t, pst[1]], [hidT[r].t], out=hidT[r].ap[:, fc, :], in0=sg[fc].ap, in1=psb[1][:, 0:CAP], op=ALU.mult)
        for s_ in range(2):
            yb = ysb[s_]
            for half in range(2):
                bk = 3 + half
                for fc in range(2):
                    OP('pe', 'matmul', [hidT[r].t, wdb[r].t], [pst[bk]], psb[bk], lhsT=hidT[r].ap[:, fc, s_ * 128:(s_ + 1) * 128], rhs=wdb[r].ap[:, fc, half * 512:(half + 1) * 512], start=(fc == 0), stop=(fc == 1))
                OP('act' if half == 0 else 'dve', 'activation' if half == 0 else 'tensor_copy', [pst[bk]], [yb.t], out=yb.ap[:, half * 512:(half + 1) * 512], in_=psb[bk], **({'func': AF.Copy} if half == 0 else {}))
            t_ys.append(T('ys'))
            OP('sp', 'dma_start', [yb.t], [t_ys[-1]], out=ys_scr[e * CAP + s_ * 128:e * CAP + (s_ + 1) * 128, :], in_=yb.ap, dma='ysst%d' % s_)

    stage[0] = 'P3'
    P3R = opt.get('p3ring', 4)
    y0 = [V(zs_r[i // 2].ap[:, 0, :], zs_r[i // 2].t, (i % 2) * D, (i % 2 + 1) * D, [128, D], BF16) for i in range(4)][:P3R]
    y1 = [V(xs_tm_r[i // 2].ap[:, 0, :], xs_tm_r[i // 2].t, (i % 2) * D, (i % 2 + 1) * D, [128, D], BF16) for i in range(4)][:P3R]
    x2b = [V(gsg_r[0].ap, gsg_r[0].t, 0, 2048, [128, D], F32), V(gsg_r[1].ap, gsg_r[1].t, 0, 2048, [128, D], F32),
           V(xt_r[1].ap[:, 0, :], xt_r[1].t, 0, D, [128, D], F32), V(h2T.ap.rearrange("p a b -> p (a b)"), h2T.t, 0, D, [128, D], F32)][:P3R]
    t_fin = T("fin")
    for ci in range(nsc * NCH):
        r = ci % P3R
        OP('pool', 'indirect_dma_start', t_ys + [slots_i.t], [y0[r].t], out=y0[r].ap, out_offset=None, in_=ys_scr, in_offset=bass.IndirectOffsetOnAxis(ap=slots_i.ap[:, ci, 0:1], axis=0), dma='ga%d' % r)
        OP('pool', 'indirect_dma_start', t_ys + [slots_i.t], [y1[r].t], out=y1[r].ap, out_offset=None, in_=ys_scr, in_offset=bass.IndirectOffsetOnAxis(ap=slots_i.ap[:, ci, 1:2], axis=0), dma='gb%d' % r)
        OP('sp', 'dma_start', [t_out[ci]], [x2b[r].t], out=x2b[r].ap, in_=out_d[ci * 128:(ci + 1) * 128, :], dma='x2ld%d' % r)
        OP('dve', 'scalar_tensor_tensor', [y0[r].t, gates.t, x2b[r].t], [x2b[r].t], out=x2b[r].ap, in0=y0[r].ap, scalar=gates.ap[:, ci, 0:1], in1=x2b[r].ap, op0=ALU.mult, op1=ALU.add)
        OP('dve', 'scalar_tensor_tensor', [y1[r].t, gates.t, x2b[r].t], [x2b[r].t], out=x2b[r].ap, in0=y1[r].ap, scalar=gates.ap[:, ci, 1:2], in1=x2b[r].ap, op0=ALU.mult, op1=ALU.add)
        OP('sp', 'dma_start', [x2b[r].t], [t_out[ci]], out=out_d[ci * 128:(ci + 1) * 128, :], in_=x2b[r].ap, dma='fin%d' % r)
    tl = [t_fin] + [t_out[i] for i in range(NCHUNK)] + list(dbg_outs.values())
    S.final_wait('sp', tl)
    if model_only:
        S.schedule()
        return S
    S.run()
    return nc


def prep_core(inp, b):
    f = lambda a: np.ascontiguousarray(a, dtype=np.float32)
    rowc = np.concatenate([
        inp['dt_bias'][0], inp['a_log'][0], inp['d_skip'][0],
        np.tile(inp['q_norm_g'][0], 16), np.tile(inp['k_norm_g'][0], 4),
        inp['sinks'][0], inp['norm2_g'][0],
        inp['b_router_group'][0], inp['b_router_expert'][0].reshape(-1)]).astype(np.float32)
    assert rowc.shape[0] == R_W
    wrt = np.concatenate([inp['w_router_group'][0], np.transpose(inp['w_router_expert'][0], (1, 0, 2)).reshape(D, 64)], axis=1)
    return {
        "x": f(inp['x'][b]),
        "pos": np.ascontiguousarray(inp['positions'][b].reshape(NCHUNK, 128).T.astype(np.int32)),
        "w_in": f(inp['w_in'][0]), "w_so": f(inp['w_ssd_out'][0]), "w_ao": f(inp['w_attn_out'][0]), "w_o": f(inp['w_out'][0]),
        "g1T": f(inp['norm1_g'][0].reshape(8, 128).T),
        "convw": f(np.transpose(inp['conv_w'][0].reshape(4, 24, 128), (2, 1, 0))),
        "convb": f(inp['conv_b'][0].reshape(24, 128).T),
        "gssdT": f(inp['ssd_norm_g'][0].reshape(16, 128).T),
        "rowc": f(np.broadcast_to(rowc[None, :], (128, R_W))),
        "wr": f(np.transpose(wrt.reshape(8, 128, 72), (1, 0, 2))),
        "wg": f(inp['w_gate_e'][0]), "wu": f(inp['w_up_e'][0]), "wd": f(inp['w_down_e'][0]),
    }


def kernel(**inputs):
    inp = {k: np.asarray(v) for k, v in inputs.items()}
    nc = build()
    in_maps = [prep_core(inp, b) for b in range(8)]
    res = run_bass_kernel_spmd(nc, in_maps, core_ids=list(range(8)))
    out = np.stack([np.asarray(r["out"]).reshape(SEQ, D) for r in res.results], axis=0)
    return out.astype(np.float32)
```
